# Optimizing a Trainium2 kernel written in Bass

```python
import jax, jax.numpy as jnp
from jax import lax
import numpy as np

D_MODEL = 1024
BATCH = 2
SEQ = 16384
DEPTH = 4

RET_HEADS = 4
RET_DK = 128
RET_DV = 256
RET_CHUNK = 128
RET_QK = RET_HEADS * RET_DK
RET_VW = RET_HEADS * RET_DV
GLA_HEADS = 4
GLA_DK = 128
GLA_DV = 256
GLA_RANK = 16
GLA_TAU = 16.0
GLA_CHUNK = 64
GLA_QK = GLA_HEADS * GLA_DK
GLA_VW = GLA_HEADS * GLA_DV
SSD_INNER = 2 * D_MODEL
SSD_HEADDIM = 64
SSD_HEADS = SSD_INNER // SSD_HEADDIM
SSD_GROUPS = 4
SSD_STATE = 128
SSD_CONV = 4
SSD_CHUNK = 128
SSD_CONV_DIM = SSD_INNER + 2 * SSD_GROUPS * SSD_STATE
MOE_GROUPS = 4
MOE_PER_GROUP = 8
MOE_EXPERTS = MOE_GROUPS * MOE_PER_GROUP
MOE_TOPK = 2
MOE_FF = 512
MOE_BLOCK = 256

ROPE_BASE = 10000.0
EPS = 1e-6

SPLIT_SIZES = (RET_QK, RET_QK, RET_VW, RET_VW,
               GLA_QK, GLA_QK, GLA_VW, GLA_VW, GLA_RANK,
               SSD_INNER, SSD_CONV_DIM, SSD_HEADS,
               D_MODEL, D_MODEL, D_MODEL)
IN_WIDTH = sum(SPLIT_SIZES)
SPLIT_POINTS = tuple(int(v) for v in np.cumsum(SPLIT_SIZES)[:-1])

kernel_name = 'hybrid_ret_gla_ssd_hmoe'


def rmsnorm(x, g):
    xf = x.astype(jnp.float32)
    y = xf * lax.rsqrt(jnp.mean(xf * xf, axis=-1, keepdims=True) + EPS)
    return (y * g.astype(jnp.float32)).astype(x.dtype)


def head_layernorm(t, g):
    tf = t.astype(jnp.float32)
    mu = jnp.mean(tf, axis=-1, keepdims=True)
    c = tf - mu
    y = c * lax.rsqrt(jnp.mean(c * c, axis=-1, keepdims=True) + EPS)
    y = y * g.astype(jnp.float32).reshape(t.shape[-2:])
    return y.reshape(t.shape[:-2] + (-1,)).astype(t.dtype)


def head_rmsnorm(t, g):
    tf = t.astype(jnp.float32)
    y = tf * lax.rsqrt(jnp.mean(tf * tf, axis=-1, keepdims=True) + EPS)
    y = y * g.astype(jnp.float32).reshape(t.shape[-2:])
    return y.reshape(t.shape[:-2] + (-1,)).astype(t.dtype)


def rotary(t, pos):
    half = t.shape[-1] // 2
    inv = ROPE_BASE ** (-jnp.arange(half, dtype=jnp.float32) / half)
    ang = pos.astype(jnp.float32)[..., None] * inv
    cos = jnp.cos(ang)[:, :, None, :]
    sin = jnp.sin(ang)[:, :, None, :]
    t1 = t[..., :half].astype(jnp.float32)
    t2 = t[..., half:].astype(jnp.float32)
    return jnp.concatenate([t1 * cos - t2 * sin, t2 * cos + t1 * sin], axis=-1).astype(t.dtype)


def chunk_scan(decay, inc):
    def step(s, xs):
        d, a = xs
        return d * s + a, s
    _, prev = lax.scan(step, jnp.zeros_like(inc[0]), (decay, inc))
    return prev


def retention(q, k, v, log_gamma):
    B, S, H, dk = q.shape
    dv = v.shape[-1]
    C = RET_CHUNK
    n = S // C
    q = q.reshape(B, n, C, H, dk)
    k = k.reshape(B, n, C, H, dk)
    v = v.reshape(B, n, C, H, dv)
    idx = jnp.arange(C, dtype=jnp.float32)
    diff = idx[:, None] - idx[None, :]
    causal = diff >= 0
    dmat = jnp.where(causal[None], jnp.exp(jnp.where(causal, diff, 0.0)[None] * log_gamma[:, None, None]), 0.0)
    scores = jnp.einsum('bnihd,bnjhd->bnhij', q, k) * dmat.astype(q.dtype)
    o = jnp.einsum('bnhij,bnjhe->bnihe', scores, v)
    kdec = jnp.exp((C - 1 - idx)[:, None] * log_gamma[None]).astype(k.dtype)
    kv = jnp.einsum('bnjhd,bnjhe->nbhde', k * kdec[None, None, :, :, None], v)
    cdec = jnp.broadcast_to(jnp.exp(C * log_gamma)[None, None, :, None, None], (n, 1, H, 1, 1)).astype(kv.dtype)
    prev = chunk_scan(cdec, kv)
    qdec = jnp.exp((idx + 1)[:, None] * log_gamma[None]).astype(q.dtype)
    o = o + jnp.einsum('bnihd,nbhde->bnihe', q * qdec[None, None, :, :, None], prev)
    return o.reshape(B, S, H, dv)


def gla(q, k, v, log_a):
    B, S, H, dk = q.shape
    dv = v.shape[-1]
    C = GLA_CHUNK
    n = S // C
    q = q.reshape(B, n, C, H, dk)
    k = k.reshape(B, n, C, H, dk)
    v = v.reshape(B, n, C, H, dv)
    b = jnp.cumsum(log_a.astype(jnp.float32).reshape(B, n, C, H, dk), axis=2)
    b_ref = b[:, :, C // 2:C // 2 + 1]
    b_last = b[:, :, -1:]
    qi = q * jnp.exp(b - b_ref).astype(q.dtype)
    ki = k * jnp.exp(b_ref - b).astype(k.dtype)
    causal = jnp.tril(jnp.ones((C, C), dtype=bool))
    scores = jnp.where(causal, jnp.einsum('bnihd,bnjhd->bnhij', qi, ki), 0.0).astype(v.dtype)
    o = jnp.einsum('bnhij,bnjhe->bnihe', scores, v)
    ks = k * jnp.exp(b_last - b).astype(k.dtype)
    kv = jnp.einsum('bnjhd,bnjhe->nbhde', ks, v)
    dec = jnp.exp(b_last[:, :, 0]).transpose(1, 0, 2, 3)[..., None].astype(kv.dtype)
    prev = chunk_scan(dec, kv)
    o = o + jnp.einsum('bnihd,nbhde->bnihe', q * jnp.exp(b).astype(q.dtype), prev)
    return o.reshape(B, S, H, dv)


def ssd_scan(x, dt, a, bm, cm):
    B, S, H, P = x.shape
    G, N = bm.shape[2], bm.shape[3]
    R = H // G
    C = SSD_CHUNK
    n = S // C
    acs = jnp.cumsum((dt * a).reshape(B, n, C, H), axis=2)
    xdt = (x.astype(jnp.float32) * dt[..., None]).astype(x.dtype).reshape(B, n, C, G, R, P)
    bc = bm.reshape(B, n, C, G, N)
    cc = cm.reshape(B, n, C, G, N)
    at = acs.transpose(0, 1, 3, 2)
    mask = jnp.tril(jnp.ones((C, C), dtype=bool))
    seg = at[..., :, None] - at[..., None, :]
    lmat = jnp.exp(jnp.where(mask, seg, -jnp.inf)).reshape(B, n, G, R, C, C).astype(x.dtype)
    cb = jnp.einsum('bnigs,bnjgs->bngij', cc, bc)
    y = jnp.einsum('bngrij,bnjgrp->bnigrp', lmat * cb[:, :, :, None], xdt)
    dstates = jnp.exp(acs[:, :, -1:, :] - acs).reshape(B, n, C, G, R).astype(x.dtype)
    states = jnp.einsum('bnjgs,bnjgrp->nbgrps', bc, xdt * dstates[..., None])
    cdec = jnp.exp(acs[:, :, -1, :]).reshape(B, n, G, R).transpose(1, 0, 2, 3)[..., None, None].astype(states.dtype)
    prev = chunk_scan(cdec, states)
    dout = jnp.exp(acs).reshape(B, n, C, G, R).astype(x.dtype)
    y = y + jnp.einsum('bnigs,nbgrps->bnigrp', cc, prev) * dout[..., None]
    return y.reshape(B, S, H, P)


def causal_depthwise_conv(x, w, b):
    K, ch = w.shape
    y = lax.conv_general_dilated(x, w[:, None, :].astype(x.dtype), window_strides=(1,),
                                 padding=[(K - 1, 0)], dimension_numbers=('NWC', 'WIO', 'NWC'),
                                 feature_group_count=ch)
    return y + b.astype(x.dtype)


def hier_moe(h, w_rg, b_rg, w_re, b_re, w_g, w_u, w_d):
    T, D = h.shape
    E = w_g.shape[0]
    grp_logits = jnp.dot(h, w_rg).astype(jnp.float32) + b_rg.astype(jnp.float32)
    p_grp = jax.nn.softmax(grp_logits, axis=-1)
    g_sel = jnp.argmax(grp_logits, axis=-1).astype(jnp.int32)
    p_sel = jnp.take_along_axis(p_grp, g_sel[:, None], axis=-1)
    e_logits = (jnp.dot(h, w_re).astype(jnp.float32) + b_re.astype(jnp.float32)).reshape(T, MOE_GROUPS, MOE_PER_GROUP)
    in_grp = jnp.take_along_axis(e_logits, g_sel[:, None, None], axis=1)[:, 0]
    top_v, top_i = lax.top_k(in_grp, MOE_TOPK)
    gate = jax.nn.softmax(top_v, axis=-1) * p_sel
    eid = (g_sel[:, None] * MOE_PER_GROUP + top_i).reshape(-1).astype(jnp.int32)
    wts = gate.reshape(-1)
    A = T * MOE_TOPK
    s_eid, order = lax.sort((eid, jnp.arange(A, dtype=jnp.int32)), num_keys=1, is_stable=True)
    s_tok = order // MOE_TOPK
    s_w = wts[order]
    counts = jnp.bincount(eid, length=E).astype(jnp.int32)
    starts = jnp.cumsum(counts) - counts
    pcounts = (counts + MOE_BLOCK - 1) // MOE_BLOCK * MOE_BLOCK
    pends = jnp.cumsum(pcounts)
    pstarts = pends - pcounts
    dest = pstarts[s_eid] + jnp.arange(A, dtype=jnp.int32) - starts[s_eid]
    n_blocks = -(-A // MOE_BLOCK) + E
    P = n_blocks * MOE_BLOCK
    x_buf = jnp.zeros((P, D), h.dtype).at[dest].set(h[s_tok])
    t_buf = jnp.full((P,), T, jnp.int32).at[dest].set(s_tok)
    w_buf = jnp.zeros((P,), jnp.float32).at[dest].set(s_w)
    blk_e = jnp.minimum(jnp.searchsorted(pends, jnp.arange(n_blocks, dtype=jnp.int32) * MOE_BLOCK, side='right'), E - 1).astype(jnp.int32)

    def run_block(args):
        xb, e = args
        return jnp.dot(jax.nn.silu(jnp.dot(xb, w_g[e])) * jnp.dot(xb, w_u[e]), w_d[e])

    y = lax.map(run_block, (x_buf.reshape(n_blocks, MOE_BLOCK, D), blk_e)).reshape(P, D)
    y = (y.astype(jnp.float32) * w_buf[:, None]).astype(h.dtype)
    return jax.ops.segment_sum(y, t_buf, num_segments=T + 1)[:T]


def setup_inputs(seed: int = 0) -> dict:
    key = jax.random.key(seed)
    ks = jax.random.split(key, 32)
    f32 = jnp.float32
    L = DEPTH
    D = D_MODEL
    res_scale = (2.0 * DEPTH) ** -0.5

    def nrm(k, shape, scale):
        return jax.random.normal(k, shape, f32) * scale

    x = nrm(ks[0], (BATCH, SEQ, D), 1.0)
    positions = jnp.arange(SEQ, dtype=jnp.int32)[None, :] + jax.random.randint(ks[1], (BATCH, 1), 0, 4096, dtype=jnp.int32)
    g_mix = 1.0 + nrm(ks[2], (L, D), 0.02)
    w_in = nrm(ks[3], (L, D, IN_WIDTH), D ** -0.5)
    ret_norm = 1.0 + nrm(ks[4], (L, RET_VW), 0.02)
    w_ret_out = nrm(ks[5], (L, RET_VW, D), RET_VW ** -0.5)
    gla_w_a2 = nrm(ks[6], (L, GLA_RANK, GLA_QK), GLA_RANK ** -0.5)
    gla_b_a = nrm(ks[7], (L, GLA_QK), 0.02)
    gla_norm = 1.0 + nrm(ks[8], (L, GLA_VW), 0.02)
    w_gla_out = nrm(ks[9], (L, GLA_VW, D), GLA_VW ** -0.5)
    ssd_conv_w = nrm(ks[10], (L, SSD_CONV, SSD_CONV_DIM), SSD_CONV ** -0.5)
    ssd_conv_b = nrm(ks[11], (L, SSD_CONV_DIM), 0.02)
    dt0 = jnp.exp(jax.random.uniform(ks[12], (L, SSD_HEADS), f32, float(np.log(1e-3)), float(np.log(1e-1))))
    ssd_dt_bias = dt0 + jnp.log(-jnp.expm1(-dt0))
    ssd_a_log = jnp.log(jax.random.uniform(ks[13], (L, SSD_HEADS), f32, 1.0, 16.0))
    ssd_d = 1.0 + nrm(ks[14], (L, SSD_HEADS), 0.02)
    ssd_norm = 1.0 + nrm(ks[15], (L, SSD_INNER), 0.02)
    w_ssd_out = nrm(ks[16], (L, SSD_INNER, D), SSD_INNER ** -0.5)
    w_o = nrm(ks[17], (L, D, D), D ** -0.5 * res_scale)
    g_ffn = 1.0 + nrm(ks[18], (L, D), 0.02)
    w_rg = nrm(ks[19], (L, D, MOE_GROUPS), D ** -0.5)
    b_rg = nrm(ks[20], (L, MOE_GROUPS), 0.01)
    w_re = nrm(ks[21], (L, D, MOE_EXPERTS), D ** -0.5)
    b_re = nrm(ks[22], (L, MOE_EXPERTS), 0.01)
    w_exp_gate = nrm(ks[23], (L, MOE_EXPERTS, D, MOE_FF), D ** -0.5)
    w_exp_up = nrm(ks[24], (L, MOE_EXPERTS, D, MOE_FF), D ** -0.5)
    w_exp_down = nrm(ks[25], (L, MOE_EXPERTS, MOE_FF, D), MOE_FF ** -0.5 * res_scale)
    g_final = 1.0 + nrm(ks[26], (D,), 0.02)
    return {'x': x, 'positions': positions, 'g_mix': g_mix, 'w_in': w_in,
            'ret_norm': ret_norm, 'w_ret_out': w_ret_out,
            'gla_w_a2': gla_w_a2, 'gla_b_a': gla_b_a, 'gla_norm': gla_norm, 'w_gla_out': w_gla_out,
            'ssd_conv_w': ssd_conv_w, 'ssd_conv_b': ssd_conv_b, 'ssd_dt_bias': ssd_dt_bias,
            'ssd_a_log': ssd_a_log, 'ssd_d': ssd_d, 'ssd_norm': ssd_norm, 'w_ssd_out': w_ssd_out,
            'w_o': w_o, 'g_ffn': g_ffn, 'w_rg': w_rg, 'b_rg': b_rg, 'w_re': w_re, 'b_re': b_re,
            'w_exp_gate': w_exp_gate, 'w_exp_up': w_exp_up, 'w_exp_down': w_exp_down,
            'g_final': g_final}


def reference(x, positions, g_mix, w_in, ret_norm, w_ret_out, gla_w_a2, gla_b_a, gla_norm, w_gla_out,
              ssd_conv_w, ssd_conv_b, ssd_dt_bias, ssd_a_log, ssd_d, ssd_norm, w_ssd_out, w_o,
              g_ffn, w_rg, b_rg, w_re, b_re, w_exp_gate, w_exp_up, w_exp_down, g_final):
    B, S, D = x.shape
    log_gamma = jnp.log1p(-jnp.exp2(-5.0 - jnp.arange(RET_HEADS, dtype=jnp.float32)))
    for l in range(DEPTH):
        h = rmsnorm(x, g_mix[l])
        u = jnp.dot(h, w_in[l])
        (rq, rk, rv, rg, gq, gk, gv, gr, ga, sz, sxbc, sdt, gt_a, gt_b, gt_c) = jnp.split(u, SPLIT_POINTS, axis=-1)

        q = rotary(rq.reshape(B, S, RET_HEADS, RET_DK), positions)
        k = rotary(rk.reshape(B, S, RET_HEADS, RET_DK), positions) * (RET_DK ** -0.5)
        o = retention(q, k, rv.reshape(B, S, RET_HEADS, RET_DV), log_gamma)
        o = head_layernorm(o, ret_norm[l]) * jax.nn.silu(rg)
        y_a = jnp.dot(o, w_ret_out[l])

        log_a = jax.nn.log_sigmoid((jnp.dot(ga, gla_w_a2[l]) + gla_b_a[l]).astype(jnp.float32)) / GLA_TAU
        o = gla(gq.reshape(B, S, GLA_HEADS, GLA_DK) * (GLA_DK ** -0.5),
                gk.reshape(B, S, GLA_HEADS, GLA_DK),
                gv.reshape(B, S, GLA_HEADS, GLA_DV),
                log_a.reshape(B, S, GLA_HEADS, GLA_DK))
        o = head_rmsnorm(o, gla_norm[l]) * jax.nn.silu(gr)
        y_b = jnp.dot(o, w_gla_out[l])

        xbc = jax.nn.silu(causal_depthwise_conv(sxbc, ssd_conv_w[l], ssd_conv_b[l]))
        xs, bm, cm = jnp.split(xbc, [SSD_INNER, SSD_INNER + SSD_GROUPS * SSD_STATE], axis=-1)
        dt = jax.nn.softplus(sdt.astype(jnp.float32) + ssd_dt_bias[l].astype(jnp.float32))
        a = -jnp.exp(ssd_a_log[l].astype(jnp.float32))
        xh = xs.reshape(B, S, SSD_HEADS, SSD_HEADDIM)
        y = ssd_scan(xh, dt, a, bm.reshape(B, S, SSD_GROUPS, SSD_STATE), cm.reshape(B, S, SSD_GROUPS, SSD_STATE))
        y = y + xh * ssd_d[l][:, None]
        y = y.reshape(B, S, SSD_INNER) * jax.nn.silu(sz)
        y = head_rmsnorm(y.reshape(B, S, SSD_GROUPS, SSD_INNER // SSD_GROUPS), ssd_norm[l])
        y_c = jnp.dot(y, w_ssd_out[l])

        merged = jax.nn.sigmoid(gt_a) * y_a + jax.nn.sigmoid(gt_b) * y_b + jax.nn.sigmoid(gt_c) * y_c
        x = x + jnp.dot(merged, w_o[l])

        hf = rmsnorm(x, g_ffn[l]).reshape(B * S, D)
        x = x + hier_moe(hf, w_rg[l], b_rg[l], w_re[l], b_re[l],
                         w_exp_gate[l], w_exp_up[l], w_exp_down[l]).reshape(B, S, D)
    return rmsnorm(x, g_final)
```

```python
from concourse.bass_utils import run_bass_kernel_spmd
import contextlib
import numpy as np
import concourse.bass as bass
import concourse.mybir as mybir

F32 = mybir.dt.float32
BF16 = mybir.dt.bfloat16
I32 = mybir.dt.int32
U32 = mybir.dt.uint32
AF = mybir.ActivationFunctionType
ALU = mybir.AluOpType
AX = mybir.AxisListType

ENGS = ("pe", "act", "dve", "pool", "sp")


class Buf:
    __slots__ = ("name", "w", "r", "dsem", "dcnt", "t")

    def __init__(self, name, t=None):
        self.name = name
        self.w = {}
        self.r = {}
        self.dsem = None
        self.dcnt = 0
        self.t = t


class Op:
    __slots__ = ("eng", "fn", "deps", "is_dma", "dma_tok", "needed", "ticket", "id")


class _Rec:
    def __init__(self):
        self.call = None

    def __getattr__(self, name):
        def f(*a, **k):
            self.call = (name, a, k)
            return None
        return f


def _eager(fn):
    rec = _Rec()
    fn(rec)
    c = rec.call
    assert c is not None
    return lambda e: getattr(e, c[0])(*c[1], **c[2])


class Builder:
    def __init__(self, nc):
        self.nc = nc
        self.ops = []
        self.stack = contextlib.ExitStack()
        self.bufs = []
        self.sems = {}
        self.nsem = 0
        for e in ENGS:
            self.sems[e] = self.sem("s_" + e)
        self.scopes = [self.stack]
        self.cache = {}
        self.last_op = {}
        self.bar_tokens = []
        self.bar_pending = set()

    def sem(self, name):
        self.nsem += 1
        return self.stack.enter_context(self.nc.semaphore(name))

    @contextlib.contextmanager
    def scope(self):
        st = contextlib.ExitStack()
        self.scopes.append(st)
        try:
            yield st
        finally:
            self.scopes.pop()
            st.close()

    def sb_cached(self, name, shape, dt):
        if name not in self.cache:
            self.cache[name] = self.sb(name, shape, dt)
        return self.cache[name]

    def barrier(self):
        toks = [("op", i) for i in self.last_op.values()]
        for b in self.bufs:
            if b.dsem is not None:
                toks.append(("dma", b.dsem, b.dcnt))
        self.bar_tokens = toks
        self.bar_pending = set(ENGS)
        self.cache = {}

    def sb(self, name, shape, dt):
        t = self.scopes[-1].enter_context(self.nc.sbuf_tensor("sb_" + name, list(shape), dt))
        b = Buf(name, t)
        self.bufs.append(b)
        return b

    def ps(self, name, shape, dt=F32):
        t = self.stack.enter_context(self.nc.psum_tensor("ps_" + name, list(shape), dt))
        b = Buf(name, t)
        self.bufs.append(b)
        return b

    def dram(self, name, shape, dt, kind="Internal"):
        t = self.nc.dram_tensor(name, list(shape), dt, kind=kind)
        b = Buf(name, t)
        self.bufs.append(b)
        return b

    def track(self, name):
        b = Buf(name)
        self.bufs.append(b)
        return b

    def _op(self, eng, fn, reads, writes, is_dma=False):
        op = Op()
        op.id = len(self.ops)
        op.eng = eng
        op.fn = _eager(fn)
        op.is_dma = is_dma
        op.needed = False
        op.ticket = None
        op.dma_tok = None
        deps = []
        key_self = "e:" + eng
        for b in reads:
            for k, tok in b.w.items():
                if k == key_self and eng == "pe":
                    continue
                deps.append(tok)
        for b in writes:
            for k, tok in b.w.items():
                if k == key_self:
                    continue
                if is_dma and k == "d:" + b.name:
                    continue
                deps.append(tok)
            for k, tok in b.r.items():
                if k == key_self:
                    continue
                deps.append(tok)
        if eng in self.bar_pending:
            deps.extend(self.bar_tokens)
            self.bar_pending.discard(eng)
        op.deps = deps
        if not is_dma:
            self.last_op[eng] = op.id
        if is_dma:
            assert len(writes) == 1
            b = writes[0]
            if b.dsem is None:
                b.dsem = self.sem("d_" + b.name)
            b.dcnt += 16
            op.dma_tok = ("dma", b.dsem, b.dcnt)
            mytok = op.dma_tok
            mykey = "d:" + b.name
        else:
            mytok = ("op", op.id)
            mykey = key_self
        for b in reads:
            if b in writes:
                continue
            b.r[mykey] = mytok
        for b in writes:
            b.w = {mykey: mytok}
            b.r = {}
        self.ops.append(op)
        return op

    def op(self, eng, fn, reads=(), writes=()):
        return self._op(eng, fn, list(reads), list(writes))

    def dma(self, eng, out_ap, in_ap, reads, write, **kw):
        def fn(e):
            return e.dma_start(out=out_ap, in_=in_ap, **kw)
        return self._op(eng, fn, list(reads), [write], is_dma=True)

    def dma_fn(self, eng, fn, reads, write):
        return self._op(eng, fn, list(reads), [write], is_dma=True)

    def finish(self, final_bufs):
        ops = self.ops
        for op in ops:
            for tok in op.deps:
                if tok[0] == "op":
                    ops[tok[1]].needed = True
        cnt = {e: 0 for e in ENGS}
        for op in ops:
            if op.is_dma:
                continue
            if op.needed:
                cnt[op.eng] += 1
                op.ticket = cnt[op.eng]
        per = {e: [] for e in ENGS}
        for op in ops:
            per[op.eng].append(op)
        sems = self.sems
        final_waits = []
        for b in final_bufs:
            if b.dsem is not None:
                final_waits.append((b.dsem, b.dcnt))

        def emit_stream(engname, e):
            seen = {}
            for op in per[engname]:
                need = {}
                for tok in op.deps:
                    if tok[0] == "op":
                        p = ops[tok[1]]
                        s, v = sems[p.eng], p.ticket
                    else:
                        s, v = tok[1], tok[2]
                    if need.get(s.name, (None, 0))[1] < v:
                        need[s.name] = (s, v)
                for nm, (s, v) in need.items():
                    if seen.get(nm, 0) >= v:
                        continue
                    e.wait_ge(s, v)
                    seen[nm] = v
                ins = op.fn(e)
                if op.is_dma:
                    ins.then_inc(op.dma_tok[1], 16)
                elif op.needed:
                    ins.then_inc(sems[engname], 1)
            if engname == "sp":
                for s, v in final_waits:
                    e.wait_ge(s, v)

        nc = self.nc
        with nc.Block() as block:
            @block.tensor
            def _(e):
                emit_stream("pe", e)

            @block.scalar
            def _(e):
                emit_stream("act", e)

            @block.vector
            def _(e):
                emit_stream("dve", e)

            @block.gpsimd
            def _(e):
                emit_stream("pool", e)

            @block.sync
            def _(e):
                emit_stream("sp", e)
        self.stack.close()
        return {e: len(per[e]) for e in ENGS}


import math

D = 1024
INW = 14384
C_RQ, C_RK, C_RV, C_RG = 0, 512, 1024, 2048
C_GQ, C_GK, C_GV, C_GR, C_GA = 3072, 3584, 4096, 5120, 6144
C_SZ, C_XBC, C_DT = 6160, 8208, 11280
C_GTA, C_GTB, C_GTC = 11312, 12336, 13360
EPS = 1e-6
LOG_GAMMA = [math.log1p(-2.0 ** (-5.0 - h)) for h in range(4)]
REF = 64


def host_consts():
    p = np.arange(128)
    cols = {}
    inv = (10000.0 ** (-(np.arange(64, dtype=np.float32)) / np.float32(64))).astype(np.float32)
    cols["inv"] = np.concatenate([inv, inv])[:, None]
    i = np.arange(128, dtype=np.float64)
    e1 = np.stack([np.exp((i + 1 - REF) * LOG_GAMMA[h]) for h in range(4)])
    e2 = np.stack([np.exp((REF - (i + 1)) * LOG_GAMMA[h]) * 128 ** -0.5 for h in range(4)])
    cols["e1"] = np.broadcast_to(e1.reshape(1, 512), (128, 512))
    cols["e2"] = np.broadcast_to(e2.reshape(1, 512), (128, 512))
    cin = np.array([math.exp(REF * g) for g in LOG_GAMMA])
    cdec = np.array([math.exp(128 * g) for g in LOG_GAMMA])
    ckv = np.array([math.exp((128 - REF) * g) for g in LOG_GAMMA])
    cols["cin"] = np.broadcast_to(cin[None], (128, 4))
    cols["cdec"] = np.broadcast_to(cdec[None], (128, 4))
    cols["ckv"] = np.broadcast_to(ckv[None], (128, 4))
    cols["maskT"] = (p[:, None] <= p[None, :]).astype(np.float64)
    cols["SU"] = (p[:, None] > p[None, :]).astype(np.float64)
    cols["ones"] = np.ones((128, 128))
    cols["ident"] = np.eye(128)
    sgn = np.where(p < 64, 1.0, -1.0)[:, None]
    cols["sgn"] = sgn
    off = {}
    arrs = []
    o = 0
    for k, v in cols.items():
        v = np.asarray(v, dtype=np.float64)
        off[k] = (o, v.shape[1])
        o += v.shape[1]
        arrs.append(v)
    return np.concatenate(arrs, axis=1).astype(np.float32), off


CONSTS, COFF = host_consts()
NCONST = CONSTS.shape[1]


def build(NTOK, do_moe=True, dbg=False):
    nc = bass.Bass("TRN2", target_bir_lowering=False)
    B = Builder(nc)
    NSC = NTOK // 512
    din = lambda n, s, dt=F32: B.dram(n, s, dt, kind="ExternalInput")
    dout = lambda n, s, dt=F32: B.dram(n, s, dt, kind="ExternalOutput")
    x_d = din("x", [NTOK, D])
    pos_d = din("pos", [1, NTOK], I32)
    consts_d = din("consts", [128, NCONST])
    w_in_d = din("w_in", [D, INW])
    w_ro_d = din("w_ret_out", [1024, D])
    w_go_d = din("w_gla_out", [1024, D])
    w_so_d = din("w_ssd_out", [2048, D])
    w_o_d = din("w_o", [D, D])
    vec_d = din("vecs", [1, 1024 + 96])
    nw_d = din("normw", [128, 48])
    gba_d = din("gla_b_a", [128, 4])
    wa2_d = din("gla_w_a2", [16, 512])
    convw_d = din("convw", [128, 24 * 4])
    convb_d = din("convb", [128, 24])
    s_ret_d = din("s_ret", [128, 1024])
    s_gla_d = din("s_gla", [128, 1024])
    s_ssd_d = din("s_ssd", [128, 2048])
    hist_d = din("hist", [128, 24 * 3])
    wr_d = din("w_router", [D, 36])
    br_d = din("b_router", [1, 36])
    wg_d = din("w_exp_gate", [32, D, 512])
    wu_d = din("w_exp_up", [32, D, 512])
    wd_d = din("w_exp_down", [32, 512, D])
    xo_d = dout("x_out", [NTOK, D])
    xn_d = dout("xn_out", [NTOK, D])
    so_ret_d = dout("so_ret", [128, 1024])
    so_gla_d = dout("so_gla", [128, 1024])
    so_ssd_d = dout("so_ssd", [128, 2048])
    ho_d = dout("hist_out", [128, 24 * 3])
    outs = [xo_d, xn_d, so_ret_d, so_gla_d, so_ssd_d, ho_d]
    if dbg:
        dbg_d = dout("dbg", [128, 16384])
        outs.append(dbg_d)

    cst = B.sb("cst", [128, NCONST], F32)
    B.dma("sp", cst.t[:, :], consts_d.t[:, :], [consts_d], cst)

    def C(name, a=0, b=None):
        o, n = COFF[name]
        b = n if b is None else b
        return cst.t[:, o + a:o + b]
    identb = B.sb("identb", [128, 128], BF16)
    B.op("dve", lambda e: e.tensor_copy(identb.t[:, :], C("ident")), [cst], [identb])
    vecs = B.sb("vecs", [128, 1024 + 96], F32)
    normw = B.sb("normw", [128, 48], F32)
    B.dma("sp", normw.t[:, :], nw_d.t[:, :], [nw_d], normw)
    B.dma("sp", vecs.t[:, :], vec_d.t[0:1, :].partition_broadcast(128), [vec_d], vecs)
    V_GFIN, V_DTB, V_ALOG, V_DD = 0, 1024, 1056, 1088
    V_GMIX, V_RETN, V_GLAN, V_SSDN, V_GFFN = 0, 8, 16, 24, 40
    gba = B.sb("gba", [128, 4], F32)
    B.dma("sp", gba.t[:, :], gba_d.t[:, :], [gba_d], gba)
    wa2f = B.sb("wa2f", [16, 512], F32)
    B.dma("sp", wa2f.t[:, :], wa2_d.t[:, :], [wa2_d], wa2f)
    wa2 = B.sb("wa2", [16, 512], BF16)
    B.op("dve", lambda e: e.tensor_copy(wa2.t[:, :], wa2f.t[:, :]), [wa2f], [wa2])
    convw = B.sb("convw", [128, 96], F32)
    convb = B.sb("convb", [128, 24], F32)
    B.dma("sp", convw.t[:, :], convw_d.t[:, :], [convw_d], convw)
    B.dma("sp", convb.t[:, :], convb_d.t[:, :], [convb_d], convb)
    S_ret = B.sb("S_ret", [128, 1024], F32)
    S_gla = B.sb("S_gla", [128, 1024], F32)
    S_ssd = B.sb("S_ssd", [128, 2048], F32)
    hist = B.sb("hist", [128, 72], F32)
    B.dma("sp", S_ret.t[:, :], s_ret_d.t[:, :], [s_ret_d], S_ret)
    B.dma("sp", S_gla.t[:, :], s_gla_d.t[:, :], [s_gla_d], S_gla)
    B.dma("sp", S_ssd.t[:, :], s_ssd_d.t[:, :], [s_ssd_d], S_ssd)
    B.dma("sp", hist.t[:, :], hist_d.t[:, :], [hist_d], hist)
    nega = B.sb("nega", [128, 32], F32)
    B.op("act", lambda e: e.activation(nega.t[:, :], vecs.t[:, V_ALOG:V_ALOG + 32], AF.Exp), [vecs], [nega])
    B.op("dve", lambda e: e.tensor_scalar(nega.t[:, :], nega.t[:, :], -1.0, None, ALU.mult), [nega], [nega])

    cosT = B.sb("cosT", [128, 512], BF16)
    ssT = B.sb("ssT", [128, 512], BF16)
    posi = B.sb("posi", [128, 512], I32)
    ang = B.sb("ang", [128, 512], F32)
    ki = posi
    kf = B.sb("kf", [128, 512], F32)

    def rot_tables(t0):
        B.dma("sp", posi.t[:, :], pos_d.t[0:1, t0:t0 + 512].partition_broadcast(128), [pos_d], posi)
        B.op("dve", lambda e: e.tensor_copy(ang.t[:, :], posi.t[:, :]), [posi], [ang])
        B.op("dve", lambda e: e.tensor_scalar(ang.t[:, :], ang.t[:, :], C("inv"), None, ALU.mult), [ang, cst], [ang])
        B.op("dve", lambda e: e.tensor_scalar(ki.t[:, :], ang.t[:, :], 1.0 / (2 * math.pi), None, ALU.mult), [ang], [ki])
        B.op("dve", lambda e: e.tensor_copy(kf.t[:, :], ki.t[:, :]), [ki], [kf])
        C1 = 6.28125
        C2 = 2 * math.pi - C1
        B.op("dve", lambda e: e.scalar_tensor_tensor(ang.t[:, :], kf.t[:, :], -C1, ang.t[:, :], ALU.mult, ALU.add), [kf, ang], [ang])
        B.op("dve", lambda e: e.scalar_tensor_tensor(ang.t[:, :], kf.t[:, :], -C2, ang.t[:, :], ALU.mult, ALU.add), [kf, ang], [ang])
        B.op("dve", lambda e: e.tensor_scalar(ang.t[:, :], ang.t[:, :], math.pi, -math.pi, ALU.min, ALU.max), [ang], [ang])
        B.op("act", lambda e: e.activation(kf.t[:, :], ang.t[:, :], AF.Sin), [ang], [kf])
        B.op("dve", lambda e: e.tensor_scalar(ssT.t[:, :], kf.t[:, :], C("sgn"), None, ALU.mult), [kf, cst], [ssT])
        B.op("dve", lambda e: e.tensor_scalar(kf.t[:, :], ang.t[:, :], -1.0, None, ALU.mult), [ang], [kf])
        B.op("dve", lambda e: e.tensor_tensor(ang.t[:, :], ang.t[:, :], kf.t[:, :], ALU.max), [ang, kf], [ang])
        B.op("dve", lambda e: e.tensor_scalar(ang.t[:, :], ang.t[:, :], -1.0, math.pi / 2, ALU.mult, ALU.add), [ang], [ang])
        B.op("act", lambda e: e.activation(cosT.t[:, :], ang.t[:, :], AF.Sin), [ang], [cosT])

    pA = B.ps("pA", [128, 512])
    pB = B.ps("pB", [128, 512])
    pS = B.ps("pS", [128, 512])
    pT = B.ps("pT", [128, 1024], BF16)
    pO = B.ps("pO", [128, 1024])
    pK = B.ps("pK", [128, 1024])
    prot = [pA, pB]
    pcnt = [0]

    def nextp():
        pcnt[0] += 1
        return prot[pcnt[0] % 2]

    NWB = 2
    wts = [B.sb("wt%d" % i, [128, 8, 512], BF16) for i in range(NWB)]
    wcnt = [0]

    def load_w(src_d, r0, c0, ncols, nk=8):
        wt = wts[wcnt[0] % NWB]
        wcnt[0] += 1
        src = src_d.t[r0:r0 + nk * 128, c0:c0 + ncols].rearrange("(k p) n -> p k n", p=128)
        B.dma("pool", wt.t[:, 0:nk, 0:ncols], src, [src_d], wt)
        return wt

    msc = B.scope()
    msc.__enter__()
    xs_t = [B.sb("xs%d" % i, [128, D], F32) for i in range(2)]
    hT = B.sb("hT", [128, 8, 512], BF16)
    stat = B.sb("stat", [128, 64], F32)
    mergedT = B.sb("mergedT", [128, 8, 512], BF16)
    qT = B.sb("qT", [128, 4, 512], BF16)
    kT = B.sb("kT", [128, 4, 512], BF16)
    vtm = [B.sb("vtm%d" % i, [128, 1024], BF16) for i in range(4)]
    gtm = [B.sb("gtm%d" % i, [128, 2048], BF16) for i in range(4)]
    oT = B.sb("oT", [128, 16, 512], BF16)
    tmpf = B.sb("tmpf", [128, 512], F32)
    cum = B.sb("cum", [128, 512], F32)
    scm = B.sb("scm", [128, 512], BF16)
    ktm = B.sb("ktm", [128, 512], BF16)
    Sb = B.sb("Sb", [128, 2048], BF16)
    of = B.sb("of", [128, 2048], F32)
    sq = B.sb("sq", [128, 2048], F32)
    gcs = B.sb("gcs", [128, 48], F32)
    gbias = B.sb("gbias", [128, 32], F32)
    xbc = B.sb("xbc", [128, 24, 512], BF16)
    class _V:
        def __init__(self, ap): self.t = ap
    e1a_t = xbc.t[:, 0:4, :]
    e2a_t = xbc.t[:, 4:8, :]
    xc = B.sb("xc", [128, 515], F32)
    acc = B.sb("acc", [128, 512], F32)
    tmpf2 = acc
    dtt = B.sb("dtt", [128, 4, 32], F32)
    dts = B.sb("dts", [128, 160], F32)
    ex3 = B.sb("ex3", [128, 96], F32)
    xstm = B.sb("xstm", [128, 2048], BF16)
    xdt = B.sb("xdt", [128, 2048], BF16)
    xdtd = B.sb("xdtd", [128, 2048], BF16)
    ob = xdtd
    hb = xdt
    junk = sq
    btm = B.sb("btm", [128, 512], BF16)
    cbm = B.sb("cbm", [128, 512], F32)
    lh = B.sb("lh", [128, 512], F32)
    el = B.sb("el", [128, 512], F32)
    lcb = B.sb("lcb", [128, 512], BF16)

    dbgcol = [0]

    def dbg_dump(ap_fn, buf, ncol):
        if not dbg:
            return
        c0 = dbgcol[0]
        dbgcol[0] += ncol
        stg = B.sb_cached("dbgs", [128, 512], F32)
        B.op("dve", lambda e: e.tensor_copy(stg.t[:, :], ap_fn()), [buf], [stg])
        B.dma("sp", dbg_d.t[:, c0:c0 + ncol], stg.t[:, :], [stg], dbg_d)

    def rmsnorm_tile(xt, gcol, out_bf, scale_extra=None):
        B.op("act", lambda e: e.activation(junk.t[:, 0:D], xt.t[:, :], AF.Square, accum_out=stat.t[:, 0:1]), [xt], [junk, stat])
        B.op("dve", lambda e: e.tensor_scalar(stat.t[:, 1:2], stat.t[:, 0:1], 1.0 / D, EPS, ALU.mult, ALU.add), [stat], [stat])
        B.op("act", lambda e: e.activation(stat.t[:, 2:3], stat.t[:, 1:2], AF.Ln), [stat], [stat])
        B.op("act", lambda e: e.activation(stat.t[:, 2:3], stat.t[:, 2:3], AF.Exp, scale=-0.5), [stat], [stat])
        B.op("dve", lambda e: e.tensor_scalar(out_bf.t[:, 0:D], xt.t[:, :], stat.t[:, 2:3], None, ALU.mult), [xt, stat], [out_bf])

    def transpose_to(dst_fn, src_bf, nblk, dstbuf, wcol):
        for k0 in range(0, nblk, 4):
            nb = min(4, nblk - k0)
            for k in range(nb):
                B.op("pe", lambda e, k=k: e.transpose(pT.t[:, k * 128:(k + 1) * 128], src_bf.t[:, (k0 + k) * 128:(k0 + k + 1) * 128], identb.t[:, :]), [src_bf, identb], [pT])
            for k in range(nb):
                B.op("act", lambda e, k=k: e.activation(dst_fn(k0 + k), pT.t[:, k * 128:(k + 1) * 128], AF.Copy, scale=normw.t[:, wcol + k0 + k:wcol + k0 + k + 1]), [pT, normw], [dstbuf])

    def proj_fm(src_d, c0, ncols, evac, nk=8, rhs_fn=None, r0=0):
        for cb in range(0, ncols, 512):
            n = min(512, ncols - cb)
            wt = load_w(src_d, r0, c0 + cb, n, nk)
            for m in range(0, n, 128):
                mm = min(128, n - m)
                ps = nextp()
                for k in range(nk):
                    rhs = hT.t[:, k, :] if rhs_fn is None else rhs_fn(k)
                    B.op("pe", lambda e, k=k, m=m, mm=mm, ps=ps, wt=wt, rhs=rhs: e.matmul(ps.t[0:mm, :], wt.t[:, k, m:m + mm], rhs, start=(k == 0), stop=(k == nk - 1)),
                         [wt, hT, oT], [ps])
                evac((cb + m) // 128, ps, mm)

    def proj_tm(src_d, c0, ncols, evac, lhs_fn=None, nk=8, r0=0, lhs_buf=None):
        for cb in range(0, ncols, 512):
            n = min(512, ncols - cb)
            wt = load_w(src_d, r0, c0 + cb, n, nk)
            for c in range(4):
                ps = nextp()
                for k in range(nk):
                    lhs = hT.t[:, k, c * 128:(c + 1) * 128] if lhs_fn is None else lhs_fn(k, c)
                    B.op("pe", lambda e, k=k, n=n, ps=ps, wt=wt, lhs=lhs: e.matmul(ps.t[:, 0:n], lhs, wt.t[:, k, 0:n], start=(k == 0), stop=(k == nk - 1)),
                         [wt, hT if lhs_buf is None else lhs_buf], [ps])
                evac(cb, c, ps, n)

    def lin_attn(S, cin_fn, cdec_fn, ckv_fn, c, gated_norm):
        cs = slice(c * 128, (c + 1) * 128)
        for h in range(4):
            B.op("pe", lambda e, h=h: e.matmul(pS.t[:, h * 128:(h + 1) * 128], kT.t[:, h, cs], qT.t[:, h, cs], start=True, stop=True), [kT, qT], [pS])
        B.op("dve", lambda e: e.tensor_tensor(scm.t[:, :].rearrange("p (h i) -> p h i", h=4), pS.t[:, :].rearrange("p (h i) -> p h i", h=4),
                                               C("maskT").unsqueeze(1).to_broadcast([128, 4, 128]), ALU.mult), [pS, cst], [scm])
        for h in range(4):
            B.op("pe", lambda e, h=h: e.transpose(pT.t[:, h * 128:(h + 1) * 128], kT.t[:, h, cs], identb.t[:, :]), [kT, identb], [pT])
        B.op("act", lambda e: e.copy(ktm.t[:, :], pT.t[:, 0:512]), [pT], [ktm])
        B.op("pool", lambda e: e.tensor_tensor(Sb.t[:, 0:1024].rearrange("p (h e) -> p h e", h=4), S.t[:, :].rearrange("p (h e) -> p h e", h=4),
                                                cin_fn().unsqueeze(2).to_broadcast([128, 4, 256]), ALU.mult), [S, cst, gcs], [Sb])
        for h in range(4):
            B.op("pe", lambda e, h=h: e.matmul(pO.t[:, h * 256:(h + 1) * 256], scm.t[:, h * 128:(h + 1) * 128], vtm[c].t[:, h * 256:(h + 1) * 256], start=True, stop=False), [scm, vtm[c]], [pO])
            B.op("pe", lambda e, h=h: e.matmul(pO.t[:, h * 256:(h + 1) * 256], qT.t[:, h, cs], Sb.t[:, h * 256:(h + 1) * 256], start=False, stop=True), [qT, Sb], [pO])
        for h in range(4):
            B.op("pe", lambda e, h=h: e.matmul(pK.t[:, h * 256:(h + 1) * 256], ktm.t[:, h * 128:(h + 1) * 128], vtm[c].t[:, h * 256:(h + 1) * 256], start=True, stop=True), [ktm, vtm[c]], [pK])
        gated_norm(c)
        B.op("dve", lambda e: e.tensor_tensor(S.t[:, :].rearrange("p (h e) -> p h e", h=4), S.t[:, :].rearrange("p (h e) -> p h e", h=4),
                                               cdec_fn().unsqueeze(2).to_broadcast([128, 4, 256]), ALU.mult), [S, cst, gcs], [S])
        B.op("dve", lambda e: e.tensor_tensor(sq.t[:, 0:1024].rearrange("p (h e) -> p h e", h=4), pK.t[:, :].rearrange("p (h e) -> p h e", h=4),
                                               ckv_fn().unsqueeze(2).to_broadcast([128, 4, 256]), ALU.mult), [pK, cst, gcs], [sq])
        B.op("pool", lambda e: e.tensor_tensor(S.t[:, :], S.t[:, :], sq.t[:, 0:1024], ALU.add), [S, sq], [S])

    def head_norm(c, src_ps_fn, ncol, hd, gcol, layer_norm, okoff, gate_tile, extra_mul=None):
        nh = ncol // hd
        v3 = lambda t: t.t[:, 0:ncol].rearrange("p (h e) -> p h e", h=nh)
        st = lambda a: stat.t[:, a:a + nh]
        B.op("pool", lambda e: e.tensor_tensor(sq.t[:, 0:ncol], of.t[:, 0:ncol], of.t[:, 0:ncol], ALU.mult), [of], [sq])
        B.op("dve", lambda e: e.tensor_reduce(st(32), v3(sq), AX.X, ALU.add), [sq], [stat])
        if layer_norm:
            B.op("dve", lambda e: e.tensor_reduce(st(8), v3(of), AX.X, ALU.add), [of], [stat])
            B.op("dve", lambda e: e.tensor_scalar(st(8), st(8), 1.0 / hd, None, ALU.mult), [stat], [stat])
            B.op("dve", lambda e: e.tensor_tensor(st(16), st(8), st(8), ALU.mult), [stat], [stat])
            B.op("dve", lambda e: e.scalar_tensor_tensor(st(32), st(32), 1.0 / hd, st(16), ALU.mult, ALU.subtract), [stat], [stat])
            B.op("dve", lambda e: e.tensor_tensor(v3(of), v3(of), st(8).unsqueeze(2).to_broadcast([128, nh, hd]), ALU.subtract), [of, stat], [of])
            B.op("dve", lambda e: e.tensor_scalar(st(40), st(32), EPS, None, ALU.add), [stat], [stat])
            B.op("act", lambda e: e.activation(st(40), st(40), AF.Ln), [stat], [stat])
            B.op("act", lambda e: e.activation(st(40), st(40), AF.Exp, scale=-0.5), [stat], [stat])
        else:
            B.op("dve", lambda e: e.tensor_scalar(st(40), st(32), 1.0 / hd, EPS, ALU.mult, ALU.add), [stat], [stat])
            B.op("act", lambda e: e.activation(st(40), st(40), AF.Ln), [stat], [stat])
            B.op("act", lambda e: e.activation(st(40), st(40), AF.Exp, scale=-0.5), [stat], [stat])
        B.op("dve", lambda e: e.tensor_tensor(v3(of), v3(of), st(40).unsqueeze(2).to_broadcast([128, nh, hd]), ALU.mult), [of, stat], [of])
        if gate_tile is not None:
            B.op("dve", lambda e: e.tensor_tensor(ob.t[:, 0:ncol], of.t[:, 0:ncol], gate_tile.t[:, 0:ncol], ALU.mult), [of, gate_tile], [ob])
        else:
            B.op("dve", lambda e: e.tensor_copy(ob.t[:, 0:ncol], of.t[:, 0:ncol]), [of], [ob])
        transpose_to(lambda k: oT.t[:, okoff + k, c * 128:(c + 1) * 128], ob, ncol // 128, oT, gcol)

    for sc in range(NSC):
        t0 = sc * 512
        rot_tables(t0)
        for c in range(4):
            B.dma("sp", xs_t[c % 2].t[:, :], x_d.t[t0 + c * 128:t0 + (c + 1) * 128, :], [x_d], xs_t[c % 2])
            rmsnorm_tile(xs_t[c % 2], V_GMIX, hb)
            transpose_to(lambda k, c=c: hT.t[:, k, c * 128:(c + 1) * 128], hb, 8, hT, V_GMIX)
        if dbg and sc == 0:
            dbg_dump(lambda: hT.t[:, 0, :], hT, 512)

        def rot_evac(dst, etab):
            def f(m, ps, mm):
                h = m % 4
                ts = slice(0, 512)
                B.op("dve", lambda e: e.tensor_tensor(tmpf.t[:, :], ps.t[:, :], cosT.t[:, ts], ALU.mult), [ps, cosT], [tmpf])
                B.op("dve", lambda e: e.tensor_tensor(tmpf2.t[0:64, :], ps.t[64:128, :], ssT.t[64:128, ts], ALU.mult), [ps, ssT], [tmpf2])
                B.op("dve", lambda e: e.tensor_tensor(tmpf2.t[64:128, :], ps.t[0:64, :], ssT.t[0:64, ts], ALU.mult), [ps, ssT], [tmpf2])
                B.op("pool", lambda e: e.tensor_tensor(tmpf.t[:, :], tmpf.t[:, :], tmpf2.t[:, :], ALU.add), [tmpf, tmpf2], [tmpf])
                B.op("pool", lambda e: e.tensor_tensor(dst.t[:, h, :].rearrange("p (c i) -> p c i", c=4), tmpf.t[:, :].rearrange("p (c i) -> p c i", c=4),
                                                        C(etab, h * 128, (h + 1) * 128).unsqueeze(1).to_broadcast([128, 4, 128]), ALU.mult), [tmpf, cst], [dst])
            return f
        proj_fm(w_in_d, C_RQ, 512, rot_evac(qT, "e1"))
        proj_fm(w_in_d, C_RK, 512, rot_evac(kT, "e2"))
        if dbg and sc == 0:
            dbg_dump(lambda: qT.t[:, 0, :], qT, 512)
            dbg_dump(lambda: kT.t[:, 1, :], kT, 512)
        proj_tm(w_in_d, C_RV, 1024, lambda cb, c, ps, n: B.op("act", lambda e: e.copy(vtm[c].t[:, cb:cb + n], ps.t[:, 0:n]), [ps], [vtm[c]]))
        proj_tm(w_in_d, C_RG, 1024, lambda cb, c, ps, n: B.op("act", lambda e: e.activation(gtm[c].t[:, cb:cb + n], ps.t[:, 0:n], AF.Silu), [ps], [gtm[c]]))

        def ret_norm(c):
            B.op("act", lambda e: e.copy(of.t[:, 0:1024], pO.t[:, :]), [pO], [of])
            head_norm(c, None, 1024, 256, V_RETN, True, 0, gtm[c])
        for c in range(4):
            lin_attn(S_ret, lambda: C("cin"), lambda: C("cdec"), lambda: C("ckv"), c, ret_norm)
        if dbg and sc == 0:
            dbg_dump(lambda: oT.t[:, 0, :], oT, 512)

        def out_and_gate(w_d, nk, okoff, gcol0, first):
            gap = lambda m: vtm[m // 2].t[:, (m % 2) * 512:(m % 2 + 1) * 512]
            proj_fm(w_in_d, gcol0, 1024, lambda m, ps, mm: B.op("act", lambda e: e.activation(gap(m), ps.t[:, :], AF.Tanh, scale=0.5), [ps], [vtm[m // 2]]))
            for cb in range(0, 1024, 512):
                wtiles = [load_w(w_d, kh * 1024, cb, 512, 8) for kh in range(nk // 8)]
                for m in range(4):
                    ps = nextp()
                    for kk in range(nk):
                        wt = wtiles[kk // 8]
                        B.op("pe", lambda e, kk=kk, m=m, ps=ps, wt=wt: e.matmul(ps.t[:, :], wt.t[:, kk % 8, m * 128:(m + 1) * 128], oT.t[:, okoff + kk, :], start=(kk == 0), stop=(kk == nk - 1)), [wt, oT], [ps])
                    mi = cb // 128 + m
                    if first:
                        B.op("dve", lambda e, mi=mi, ps=ps: e.scalar_tensor_tensor(mergedT.t[:, mi, :], gap(mi), 1.0, ps.t[:, :], ALU.add, ALU.mult), [vtm[mi // 2], ps], [mergedT])
                    else:
                        B.op("dve", lambda e, mi=mi, ps=ps: e.scalar_tensor_tensor(tmpf.t[:, :], gap(mi), 1.0, ps.t[:, :], ALU.add, ALU.mult), [vtm[mi // 2], ps], [tmpf])
                        B.op("pool", lambda e, mi=mi: e.tensor_tensor(mergedT.t[:, mi, :], mergedT.t[:, mi, :], tmpf.t[:, :], ALU.add), [mergedT, tmpf], [mergedT])
        out_and_gate(w_ro_d, 8, 0, C_GTA, True)

        gaT = B.sb_cached("gaT", [16, 512], BF16)
        proj_fm(w_in_d, C_GA, 16, lambda m, ps, mm: B.op("act", lambda e: e.copy(gaT.t[:, :], ps.t[0:16, :]), [ps], [gaT]))
        for h in range(4):
            B.op("pe", lambda e, h=h: e.matmul(pS.t[:, :], wa2.t[:, h * 128:(h + 1) * 128], gaT.t[:, :], start=True, stop=True), [wa2, gaT], [pS])
            B.op("dve", lambda e, h=h: e.tensor_scalar(gbias.t[:, 0:1], gba.t[:, h:h + 1], -1.0, None, ALU.mult), [gba], [gbias])
            B.op("act", lambda e: e.activation(tmpf.t[:, :], pS.t[:, :], AF.Exp, bias=gbias.t[:, 0:1], scale=-1.0), [pS, gbias], [tmpf])
            B.op("act", lambda e: e.activation(tmpf.t[:, :], tmpf.t[:, :], AF.Ln, bias=1.0), [tmpf], [tmpf])
            for c in range(4):
                B.op("dve", lambda e, c=c: e.tensor_tensor_scan(cum.t[:, c * 128:(c + 1) * 128], tmpf.t[:, c * 128:(c + 1) * 128], tmpf.t[:, c * 128:(c + 1) * 128], 0.0, ALU.add, ALU.bypass), [tmpf], [cum])
            cum3 = cum.t[:, :].rearrange("p (c i) -> p c i", c=4)
            B.op("dve", lambda e: e.tensor_scalar(gbias.t[:, 4:8], cum3[:, :, REF - 1], 1.0 / 16, math.log(128 ** -0.5), ALU.mult, ALU.add), [cum], [gbias])
            B.op("dve", lambda e: e.tensor_scalar(gbias.t[:, 8:12], cum3[:, :, REF - 1], -1.0 / 16, None, ALU.mult), [cum], [gbias])
            gc3 = gcs.t[:, :].rearrange("p (k c h) -> p k c h", k=3, c=4)
            B.op("act", lambda e, h=h: e.activation(gc3[:, 0, :, h], cum3[:, :, REF - 1], AF.Exp, scale=-1.0 / 16), [cum], [gcs])
            B.op("act", lambda e, h=h: e.activation(gc3[:, 1, :, h], cum3[:, :, 127], AF.Exp, scale=-1.0 / 16), [cum], [gcs])
            B.op("dve", lambda e: e.tensor_tensor(gbias.t[:, 12:16], cum3[:, :, 127], cum3[:, :, REF - 1], ALU.subtract), [cum], [gbias])
            B.op("act", lambda e, h=h: e.activation(gc3[:, 2, :, h], gbias.t[:, 12:16], AF.Exp, scale=-1.0 / 16), [gbias], [gcs])
            for c in range(4):
                cs = slice(c * 128, (c + 1) * 128)
                B.op("act", lambda e, c=c, cs=cs, h=h: e.activation(e1a_t[:, h, cs], cum.t[:, cs], AF.Exp, bias=gbias.t[:, 4 + c:5 + c], scale=-1.0 / 16), [cum, gbias], [xbc])
                B.op("act", lambda e, c=c, cs=cs, h=h: e.activation(e2a_t[:, h, cs], cum.t[:, cs], AF.Exp, bias=gbias.t[:, 8 + c:9 + c], scale=1.0 / 16), [cum, gbias], [xbc])
        proj_fm(w_in_d, C_GQ, 512, lambda m, ps, mm: B.op("dve", lambda e: e.tensor_tensor(qT.t[:, m, :], ps.t[:, :], e1a_t[:, m, :], ALU.mult), [ps, xbc], [qT]))
        proj_fm(w_in_d, C_GK, 512, lambda m, ps, mm: B.op("dve", lambda e: e.tensor_tensor(kT.t[:, m, :], ps.t[:, :], e2a_t[:, m, :], ALU.mult), [ps, xbc], [kT]))
        proj_tm(w_in_d, C_GV, 1024, lambda cb, c, ps, n: B.op("act", lambda e: e.copy(vtm[c].t[:, cb:cb + n], ps.t[:, 0:n]), [ps], [vtm[c]]))
        proj_tm(w_in_d, C_GR, 1024, lambda cb, c, ps, n: B.op("act", lambda e: e.activation(gtm[c].t[:, cb:cb + n], ps.t[:, 0:n], AF.Silu), [ps], [gtm[c]]))

        def gla_norm(c):
            B.op("act", lambda e: e.copy(of.t[:, 0:1024], pO.t[:, :]), [pO], [of])
            head_norm(c, None, 1024, 256, V_GLAN, False, 0, gtm[c])
        gc3 = gcs.t[:, :].rearrange("p (k c h) -> p k c h", k=3, c=4)
        for c in range(4):
            lin_attn(S_gla, lambda c=c: gc3[:, 0, c, :], lambda c=c: gc3[:, 1, c, :], lambda c=c: gc3[:, 2, c, :], c, gla_norm)
        if dbg and sc == 0:
            dbg_dump(lambda: oT.t[:, 0, :], oT, 512)
        out_and_gate(w_go_d, 8, 0, C_GTB, False)

        def conv_evac(m, ps, mm):
            B.op("act", lambda e: e.copy(xc.t[:, 3:515], ps.t[:, :]), [ps], [xc])
            B.op("pool", lambda e: e.tensor_copy(xc.t[:, 0:3], hist.t[:, m * 3:m * 3 + 3]), [hist], [xc])
            B.op("dve", lambda e: e.tensor_scalar(acc.t[:, :], xc.t[:, 3:515], convw.t[:, m * 4 + 3:m * 4 + 4], convb.t[:, m:m + 1], ALU.mult, ALU.add), [xc, convw, convb], [acc])
            for k in range(3):
                B.op("dve", lambda e, k=k: e.scalar_tensor_tensor(acc.t[:, :], xc.t[:, k:k + 512], convw.t[:, m * 4 + k:m * 4 + k + 1], acc.t[:, :], ALU.mult, ALU.add), [xc, convw, acc], [acc])
            B.op("pool", lambda e: e.tensor_copy(hist.t[:, m * 3:m * 3 + 3], xc.t[:, 512:515]), [xc], [hist])
            B.op("act", lambda e: e.activation(xbc.t[:, m, :], acc.t[:, :], AF.Silu), [acc], [xbc])
        proj_fm(w_in_d, C_XBC, 3072, conv_evac)
        proj_tm(w_in_d, C_SZ, 2048, lambda cb, c, ps, n: B.op("act", lambda e: e.activation(gtm[c].t[:, cb:cb + n], ps.t[:, 0:n], AF.Silu), [ps], [gtm[c]]))
        proj_tm(w_in_d, C_DT, 32, lambda cb, c, ps, n: B.op("act", lambda e: e.copy(dtt.t[:, c, :], ps.t[:, 0:32]), [ps], [dtt]))
        if dbg and sc == 0:
            dbg_dump(lambda: xbc.t[:, 0, :], xbc, 512)
            dbg_dump(lambda: xbc.t[:, 17, :], xbc, 512)
        for c in range(4):
            cs = slice(c * 128, (c + 1) * 128)
            T_, A_, DT_, DTA_ = dts.t[:, 0:32], dts.t[:, 32:64], dts.t[:, 64:96], dts.t[:, 96:128]
            B.op("dve", lambda e: e.tensor_tensor(T_, dtt.t[:, c, :], vecs.t[:, V_DTB:V_DTB + 32], ALU.add), [dtt, vecs], [dts])
            B.op("dve", lambda e: e.tensor_scalar(dts.t[:, 128:160], T_, -1.0, None, ALU.mult), [dts], [dts])
            B.op("dve", lambda e: e.tensor_tensor(A_, T_, dts.t[:, 128:160], ALU.max), [dts], [dts])
            B.op("act", lambda e: e.activation(A_, A_, AF.Exp, scale=-1.0), [dts], [dts])
            B.op("act", lambda e: e.activation(A_, A_, AF.Ln, bias=1.0), [dts], [dts])
            B.op("dve", lambda e: e.scalar_tensor_tensor(DT_, T_, 0.0, A_, ALU.max, ALU.add), [dts], [dts])
            B.op("dve", lambda e: e.tensor_tensor(DTA_, DT_, nega.t[:, :], ALU.mult), [dts, nega], [dts])
            B.op("pe", lambda e: e.matmul(pS.t[:, 0:32], C("maskT"), DTA_, start=True, stop=True), [cst, dts], [pS])
            B.op("pe", lambda e: e.matmul(pS.t[:, 32:64], C("SU"), DTA_, start=True, stop=True), [cst, dts], [pS])
            B.op("pe", lambda e: e.matmul(pS.t[:, 64:96], C("ones"), DTA_, start=True, stop=True), [cst, dts], [pS])
            B.op("act", lambda e: e.activation(ex3.t[:, :], pS.t[:, 0:96], AF.Exp), [pS], [ex3])
            for k0 in range(0, 16, 4):
                for k in range(4):
                    B.op("pe", lambda e, k=k, k0=k0: e.transpose(pT.t[:, k * 128:(k + 1) * 128], xbc.t[:, k0 + k, cs], identb.t[:, :]), [xbc, identb], [pT])
                B.op("act", lambda e, k0=k0: e.copy(xstm.t[:, k0 * 128:(k0 + 4) * 128], pT.t[:, 0:512]), [pT], [xstm])
            for k in range(4):
                B.op("pe", lambda e, k=k: e.transpose(pT.t[:, k * 128:(k + 1) * 128], xbc.t[:, 16 + k, cs], identb.t[:, :]), [xbc, identb], [pT])
            B.op("act", lambda e: e.copy(btm.t[:, :], pT.t[:, 0:512]), [pT], [btm])
            x3 = lambda t: t.t[:, :].rearrange("p (h q) -> p h q", h=32)
            B.op("dve", lambda e: e.tensor_tensor(x3(xdt), x3(xstm), DT_.unsqueeze(2).to_broadcast([128, 32, 64]), ALU.mult), [xstm, dts], [xdt])
            B.op("pool", lambda e: e.tensor_tensor(x3(xdtd), x3(xdt), ex3.t[:, 32:64].unsqueeze(2).to_broadcast([128, 32, 64]), ALU.mult), [xdt, ex3], [xdtd])
            for g in range(4):
                B.op("pe", lambda e, g=g: e.matmul(pS.t[:, g * 128:(g + 1) * 128], xbc.t[:, 16 + g, cs], xbc.t[:, 20 + g, cs], start=True, stop=True), [xbc], [pS])
            B.op("dve", lambda e: e.tensor_tensor(cbm.t[:, :].rearrange("p (g i) -> p g i", g=4), pS.t[:, :].rearrange("p (g i) -> p g i", g=4),
                                                   C("maskT").unsqueeze(1).to_broadcast([128, 4, 128]), ALU.mult), [pS, cst], [cbm])
            B.op("pool", lambda e: e.tensor_copy(Sb.t[:, :], S_ssd.t[:, :]), [S_ssd], [Sb])
            for half in range(2):
                hs = slice(half * 1024, (half + 1) * 1024)
                for hq in range(4):
                    h0 = half * 16 + hq * 4
                    g = h0 // 8
                    for j in range(4):
                        B.op("dve", lambda e, j=j, h0=h0: e.tensor_scalar(lh.t[:, j * 128:(j + 1) * 128], C("SU"), DTA_[:, h0 + j:h0 + j + 1], None, ALU.mult), [cst, dts], [lh])
                    for j in range(4):
                        B.op("pe", lambda e, j=j: e.matmul(pS.t[:, j * 128:(j + 1) * 128], lh.t[:, j * 128:(j + 1) * 128], C("maskT"), start=True, stop=True), [lh, cst], [pS])
                    B.op("act", lambda e: e.activation(el.t[:, :], pS.t[:, :], AF.Exp), [pS], [el])
                    B.op("dve", lambda e, g=g: e.tensor_tensor(lcb.t[:, :].rearrange("p (j i) -> p j i", j=4), el.t[:, :].rearrange("p (j i) -> p j i", j=4),
                                                               cbm.t[:, g * 128:(g + 1) * 128].unsqueeze(1).to_broadcast([128, 4, 128]), ALU.mult), [el, cbm], [lcb])
                    for j in range(4):
                        h = h0 + j
                        B.op("pe", lambda e, j=j, h=h: e.matmul(pO.t[:, (h - half * 16) * 64:(h - half * 16 + 1) * 64], lcb.t[:, j * 128:(j + 1) * 128], xdt.t[:, h * 64:(h + 1) * 64], start=True, stop=True), [lcb, xdt], [pO])
                for gg in range(2):
                    g = half * 2 + gg
                    B.op("pe", lambda e, g=g, gg=gg: e.matmul(pK.t[:, gg * 512:(gg + 1) * 512], xbc.t[:, 20 + g, cs], Sb.t[:, g * 512:(g + 1) * 512], start=True, stop=True), [xbc, Sb], [pK])
                y3 = lambda ap: ap.rearrange("p (h q) -> p h q", h=16)
                B.op("dve", lambda e, half=half, hs=hs: e.tensor_tensor(y3(of.t[:, hs]), y3(pK.t[:, :]), ex3.t[:, half * 16:half * 16 + 16].unsqueeze(2).to_broadcast([128, 16, 64]), ALU.mult), [pK, ex3], [of])
                B.op("dve", lambda e, hs=hs: e.tensor_tensor(of.t[:, hs], of.t[:, hs], pO.t[:, :], ALU.add), [of, pO], [of])
                B.op("pool", lambda e, half=half, hs=hs: e.tensor_tensor(y3(sq.t[:, hs]), y3(xstm.t[:, hs]), vecs.t[:, V_DD + half * 16:V_DD + half * 16 + 16].unsqueeze(2).to_broadcast([128, 16, 64]), ALU.mult), [xstm, vecs], [sq])
                B.op("pool", lambda e, hs=hs: e.tensor_tensor(of.t[:, hs], of.t[:, hs], sq.t[:, hs], ALU.add), [of, sq], [of])
                for gg in range(2):
                    g = half * 2 + gg
                    B.op("pe", lambda e, g=g, gg=gg: e.matmul(pK.t[:, gg * 512:(gg + 1) * 512], btm.t[:, g * 128:(g + 1) * 128], xdtd.t[:, g * 512:(g + 1) * 512], start=True, stop=True), [btm, xdtd], [pK])
                B.op("dve", lambda e, half=half, hs=hs: e.tensor_tensor(y3(S_ssd.t[:, hs]), y3(S_ssd.t[:, hs]), ex3.t[:, 64 + half * 16:64 + half * 16 + 16].unsqueeze(2).to_broadcast([128, 16, 64]), ALU.mult), [S_ssd, ex3], [S_ssd])
                B.op("dve", lambda e, hs=hs: e.tensor_tensor(S_ssd.t[:, hs], S_ssd.t[:, hs], pK.t[:, :], ALU.add), [S_ssd, pK], [S_ssd])
            B.op("dve", lambda e: e.tensor_tensor(of.t[:, :], of.t[:, :], gtm[c].t[:, :], ALU.mult), [of, gtm[c]], [of])
            head_norm(c, None, 2048, 512, V_SSDN, False, 0, None)
        if dbg and sc == 0:
            dbg_dump(lambda: oT.t[:, 0, :], oT, 512)
        out_and_gate(w_so_d, 16, 0, C_GTC, False)

        xrb = [of, of, sq, sq]
        xra = [of.t[:, 0:1024], of.t[:, 1024:2048], sq.t[:, 0:1024], sq.t[:, 1024:2048]]
        for c in range(4):
            B.dma("sp", xra[c], x_d.t[t0 + c * 128:t0 + (c + 1) * 128, :], [x_d], xrb[c])

        def wo_evac(cb, c, ps, n):
            B.op("dve", lambda e: e.scalar_tensor_tensor(xra[c][:, cb:cb + n], ps.t[:, 0:n], 0.5, xra[c][:, cb:cb + n], ALU.mult, ALU.add), [xrb[c], ps], [xrb[c]])
        proj_tm(w_o_d, 0, 1024, wo_evac, lhs_fn=lambda k, c: mergedT.t[:, k, c * 128:(c + 1) * 128], lhs_buf=mergedT)
        for c in range(4):
            B.dma("sp", xo_d.t[t0 + c * 128:t0 + (c + 1) * 128, :], xra[c], [xrb[c]], xo_d)
    B.dma("sp", so_ret_d.t[:, :], S_ret.t[:, :], [S_ret], so_ret_d)
    B.dma("sp", so_gla_d.t[:, :], S_gla.t[:, :], [S_gla], so_gla_d)
    B.dma("sp", so_ssd_d.t[:, :], S_ssd.t[:, :], [S_ssd], so_ssd_d)
    B.dma("sp", ho_d.t[:, :], hist.t[:, :], [hist], ho_d)
    B.barrier()
    msc.__exit__(None, None, None)

    HT = min(NTOK, 1024)
    NT = HT // 128
    BIG = 1.0e30
    xm = B.sb("xm", [128, NT, D], F32)
    hfT = B.sb("hfT", [128, 8, HT], BF16)
    hf32 = B.sb("hf32", [128, 8, 128], F32)
    xn32 = B.sb("xn32", [128, D], F32)
    wrt = B.sb("wrt", [128, 8, 36], F32)
    brb = B.sb("brb", [128, 36], F32)
    B.dma("sp", wrt.t[:, :, :], wr_d.t[:, :].rearrange("(k p) n -> p k n", p=128), [wr_d], wrt)
    B.dma("sp", brb.t[:, :], br_d.t[0:1, :].partition_broadcast(128), [br_d], brb)
    Wt = B.sb("Wt", [128, NT, 32], F32)
    rt = B.sb("rt", [128, 256], F32)
    ewg = [B.sb("ewg%d" % i, [128, 8, 512], BF16) for i in range(2)]
    ewu = [B.sb("ewu%d" % i, [128, 8, 512], BF16) for i in range(2)]
    ewd = [B.sb("ewd%d" % i, [128, 4, 1024], BF16) for i in range(2)]
    actT = B.sb("actT", [128, 4, 512], BF16)
    sil = B.sb("sil", [128, 512], F32)
    mstat = B.sb("mstat", [128, 8], F32)
    identf = C("ident")
    psl = [(pO, 0), (pO, 512), (pK, 0), (pK, 512)]
    pslc = [0]
    for hh in range(NTOK // HT):
        tb = hh * HT
        for t in range(NT):
            B.dma("sp", xm.t[:, t, :], xo_d.t[tb + t * 128:tb + (t + 1) * 128, :], [xo_d], xm)
        if not do_moe:
            break
        for t in range(NT):
            B.op("act", lambda e: e.activation(sil.t[:, :], xm.t[:, t, 0:512], AF.Square, accum_out=mstat.t[:, 0:1]), [xm], [sil, mstat])
            B.op("act", lambda e: e.activation(sil.t[:, :], xm.t[:, t, 512:1024], AF.Square, accum_out=mstat.t[:, 1:2]), [xm], [sil, mstat])
            B.op("dve", lambda e: e.tensor_tensor(mstat.t[:, 2:3], mstat.t[:, 0:1], mstat.t[:, 1:2], ALU.add), [mstat], [mstat])
            B.op("dve", lambda e: e.tensor_scalar(mstat.t[:, 2:3], mstat.t[:, 2:3], 1.0 / D, EPS, ALU.mult, ALU.add), [mstat], [mstat])
            B.op("act", lambda e: e.activation(mstat.t[:, 3:4], mstat.t[:, 2:3], AF.Ln), [mstat], [mstat])
            B.op("act", lambda e: e.activation(mstat.t[:, 3:4], mstat.t[:, 3:4], AF.Exp, scale=-0.5), [mstat], [mstat])
            B.op("dve", lambda e: e.tensor_scalar(xn32.t[:, :], xm.t[:, t, :], mstat.t[:, 3:4], None, ALU.mult), [xm, mstat], [xn32])
            for k in range(8):
                B.op("pe", lambda e: e.transpose(pO.t[:, k * 128:(k + 1) * 128], xn32.t[:, k * 128:(k + 1) * 128], identf), [xn32, cst], [pO])
            for k in range(8):
                B.op("act", lambda e: e.activation(hf32.t[:, k, :], pO.t[:, k * 128:(k + 1) * 128], AF.Copy, scale=normw.t[:, V_GFFN + k:V_GFFN + k + 1]), [pO, normw], [hf32])
            B.op("dve", lambda e: e.tensor_copy(hfT.t[:, :, t * 128:(t + 1) * 128], hf32.t[:, :, :]), [hf32], [hfT])
            for k in range(8):
                B.op("pe", lambda e: e.matmul(pS.t[:, 0:36], hf32.t[:, k, :], wrt.t[:, k, :], start=(k == 0), stop=(k == 7)), [hf32, wrt], [pS])
            R = lambda a, b: rt.t[:, a:b]
            LG, GM, GE, GS, GK, NB, EM, M1, OH1, M2, OH2, DD, W1, W2 = (0, 36), (40, 41), (44, 48), (48, 49), (52, 56), (56, 60), (64, 96), (96, 97), (100, 132), (132, 133), (136, 168), (168, 169), (170, 171), (172, 173)
            r = lambda x: rt.t[:, x[0]:x[1]]
            B.op("dve", lambda e: e.tensor_tensor(r(LG), pS.t[:, 0:36], brb.t[:, :], ALU.add), [pS, brb], [rt])
            B.op("dve", lambda e: e.tensor_reduce(r(GM), rt.t[:, 0:4], AX.X, ALU.max), [rt], [rt])
            B.op("dve", lambda e: e.tensor_scalar(rt.t[:, 41:42], r(GM), -1.0, None, ALU.mult), [rt], [rt])
            B.op("act", lambda e: e.activation(r(GE), rt.t[:, 0:4], AF.Exp, bias=rt.t[:, 41:42], accum_out=r(GS)), [rt], [rt])
            B.op("dve", lambda e: e.reciprocal(r(GS), r(GS)), [rt], [rt])
            B.op("dve", lambda e: e.tensor_scalar(r(GK), rt.t[:, 0:4], r(GM), None, ALU.is_equal), [rt], [rt])
            B.op("dve", lambda e: e.tensor_scalar(r(NB), r(GK), -1.0, BIG, ALU.add, ALU.mult), [rt], [rt])
            B.op("dve", lambda e: e.tensor_tensor(r(EM).rearrange("p (g j) -> p g j", g=4), rt.t[:, 4:36].rearrange("p (g j) -> p g j", g=4), r(NB).unsqueeze(2).to_broadcast([128, 4, 8]), ALU.add), [rt], [rt])
            B.op("dve", lambda e: e.tensor_reduce(r(M1), r(EM), AX.X, ALU.max), [rt], [rt])
            B.op("dve", lambda e: e.tensor_scalar(r(OH1), r(EM), r(M1), None, ALU.is_equal), [rt], [rt])
            B.op("dve", lambda e: e.scalar_tensor_tensor(r(EM), r(OH1), -BIG, r(EM), ALU.mult, ALU.add), [rt], [rt])
            B.op("dve", lambda e: e.tensor_reduce(r(M2), r(EM), AX.X, ALU.max), [rt], [rt])
            B.op("dve", lambda e: e.tensor_scalar(r(OH2), r(EM), r(M2), None, ALU.is_equal), [rt], [rt])
            B.op("dve", lambda e: e.tensor_tensor(r(DD), r(M2), r(M1), ALU.subtract), [rt], [rt])
            B.op("act", lambda e: e.activation(r(DD), r(DD), AF.Exp), [rt], [rt])
            B.op("dve", lambda e: e.tensor_scalar(r(W1), r(DD), 1.0, None, ALU.add), [rt], [rt])
            B.op("dve", lambda e: e.reciprocal(r(W1), r(W1)), [rt], [rt])
            B.op("dve", lambda e: e.tensor_tensor(r(W1), r(W1), r(GS), ALU.mult), [rt], [rt])
            B.op("dve", lambda e: e.tensor_tensor(r(W2), r(W1), r(DD), ALU.mult), [rt], [rt])
            B.op("dve", lambda e: e.tensor_scalar(Wt.t[:, t, :], r(OH1), r(W1), None, ALU.mult), [rt], [Wt])
            B.op("dve", lambda e: e.scalar_tensor_tensor(Wt.t[:, t, :], r(OH2), r(W2), Wt.t[:, t, :], ALU.mult, ALU.add), [rt, Wt], [Wt])
        for ex in range(32):
            wg, wu, wd = ewg[ex % 2], ewu[ex % 2], ewd[ex % 2]
            B.dma("pool", wg.t[:, :, :], wg_d.t[ex, :, :].rearrange("(k p) n -> p k n", p=128), [wg_d], wg)
            B.dma("pool", wu.t[:, :, :], wu_d.t[ex, :, :].rearrange("(k p) n -> p k n", p=128), [wu_d], wu)
            B.dma("pool", wd.t[:, :, :], wd_d.t[ex, :, :].rearrange("(k p) n -> p k n", p=128), [wd_d], wd)
            for tg in range(HT // 512):
                ts = slice(tg * 512, (tg + 1) * 512)
                for fc in range(4):
                    for k in range(8):
                        B.op("pe", lambda e: e.matmul(pA.t[:, :], wg.t[:, k, fc * 128:(fc + 1) * 128], hfT.t[:, k, ts], start=(k == 0), stop=(k == 7)), [wg, hfT], [pA])
                    for k in range(8):
                        B.op("pe", lambda e: e.matmul(pB.t[:, :], wu.t[:, k, fc * 128:(fc + 1) * 128], hfT.t[:, k, ts], start=(k == 0), stop=(k == 7)), [wu, hfT], [pB])
                    B.op("act", lambda e: e.activation(sil.t[:, :], pA.t[:, :], AF.Silu), [pA], [sil])
                    B.op("dve", lambda e: e.tensor_tensor(actT.t[:, fc, :], sil.t[:, :], pB.t[:, :], ALU.mult), [sil, pB], [actT])
                for c in range(4):
                    t = tg * 4 + c
                    for dh in range(2):
                        pb, po = psl[pslc[0] % 4]
                        pslc[0] += 1
                        for fc in range(4):
                            B.op("pe", lambda e: e.matmul(pb.t[:, po:po + 512], actT.t[:, fc, c * 128:(c + 1) * 128], wd.t[:, fc, dh * 512:(dh + 1) * 512], start=(fc == 0), stop=(fc == 3)), [actT, wd], [pb])
                        B.op("dve", lambda e: e.scalar_tensor_tensor(xm.t[:, t, dh * 512:(dh + 1) * 512], pb.t[:, po:po + 512], Wt.t[:, t, ex:ex + 1], xm.t[:, t, dh * 512:(dh + 1) * 512], ALU.mult, ALU.add), [pb, Wt, xm], [xm])
        for t in range(NT):
            B.dma("sp", xo_d.t[tb + t * 128:tb + (t + 1) * 128, :], xm.t[:, t, :], [xm], xo_d)
    for t in range(NTOK // 128):
        xt_ = B.sb_cached("fin%d" % (t % 2), [128, D], F32)
        B.dma("sp", xt_.t[:, :], xo_d.t[t * 128:(t + 1) * 128, :], [xo_d], xt_)
        B.op("act", lambda e: e.activation(sil.t[:, :], xt_.t[:, 0:512], AF.Square, accum_out=mstat.t[:, 0:1]), [xt_], [sil, mstat])
        B.op("act", lambda e: e.activation(sil.t[:, :], xt_.t[:, 512:1024], AF.Square, accum_out=mstat.t[:, 1:2]), [xt_], [sil, mstat])
        B.op("dve", lambda e: e.tensor_tensor(mstat.t[:, 2:3], mstat.t[:, 0:1], mstat.t[:, 1:2], ALU.add), [mstat], [mstat])
        B.op("dve", lambda e: e.tensor_scalar(mstat.t[:, 2:3], mstat.t[:, 2:3], 1.0 / D, EPS, ALU.mult, ALU.add), [mstat], [mstat])
        B.op("act", lambda e: e.activation(mstat.t[:, 3:4], mstat.t[:, 2:3], AF.Ln), [mstat], [mstat])
        B.op("act", lambda e: e.activation(mstat.t[:, 3:4], mstat.t[:, 3:4], AF.Exp, scale=-0.5), [mstat], [mstat])
        B.op("dve", lambda e: e.scalar_tensor_tensor(xn32.t[:, :], xt_.t[:, :], mstat.t[:, 3:4], vecs.t[:, V_GFIN:V_GFIN + D], ALU.mult, ALU.mult), [xt_, mstat, vecs], [xn32])
        B.dma("sp", xn_d.t[t * 128:(t + 1) * 128, :], xn32.t[:, :], [xn32], xn_d)
    info = B.finish(outs)
    return nc, info


_PROG = {}


def _layer_inputs(p, l, x, pos, st):
    f = np.float32
    vec = np.concatenate([p["g_final"], p["ssd_dt_bias"][l], p["ssd_a_log"][l], p["ssd_d"][l]]).astype(f)[None]
    fm = lambda v: np.ascontiguousarray(v.reshape(-1, 128).T)
    normw = np.concatenate([fm(p["g_mix"][l]), fm(p["ret_norm"][l]), fm(p["gla_norm"][l]), fm(p["ssd_norm"][l]), fm(p["g_ffn"][l])], axis=1).astype(f)
    convw = np.ascontiguousarray(p["ssd_conv_w"][l].T.reshape(24, 128, 4).transpose(1, 0, 2).reshape(128, 96)).astype(f)
    convb = fm(p["ssd_conv_b"][l]).astype(f)
    m = dict(x=x, pos=pos.reshape(1, -1).astype(np.int32), consts=CONSTS, w_in=p["w_in"][l], w_ret_out=p["w_ret_out"][l],
             w_gla_out=p["w_gla_out"][l], w_ssd_out=p["w_ssd_out"][l], w_o=p["w_o"][l], vecs=vec, normw=normw,
             gla_b_a=fm(p["gla_b_a"][l]).astype(f), gla_w_a2=p["gla_w_a2"][l], convw=convw, convb=convb,
             w_router=np.concatenate([p["w_rg"][l], p["w_re"][l]], axis=1), b_router=np.concatenate([p["b_rg"][l], p["b_re"][l]])[None],
             w_exp_gate=p["w_exp_gate"][l], w_exp_up=p["w_exp_up"][l], w_exp_down=p["w_exp_down"][l],
             s_ret=st["s_ret"], s_gla=st["s_gla"], s_ssd=st["s_ssd"], hist=st["hist"])
    return {k: np.ascontiguousarray(v) for k, v in m.items()}


def _zero_state():
    return dict(s_ret=np.zeros((128, 1024), np.float32), s_gla=np.zeros((128, 1024), np.float32),
                s_ssd=np.zeros((128, 2048), np.float32), hist=np.zeros((128, 72), np.float32))


def run_pipeline(p, NTOK, CPS, DEPTH, NB):
    if NTOK not in _PROG:
        _PROG[NTOK] = build(NTOK, do_moe=True)[0]
    nc = _PROG[NTOK]
    x = np.asarray(p["x"], dtype=np.float32)
    pos = np.asarray(p["positions"])
    X = {(b, c): np.ascontiguousarray(x[b, c * NTOK:(c + 1) * NTOK]) for b in range(NB) for c in range(CPS)}
    ST = {}
    out = np.zeros_like(x)
    zeros_x = np.zeros((NTOK, 1024), np.float32)
    for n in range(DEPTH + CPS - 1):
        in_maps = []
        active = []
        for b in range(NB):
            for c in range(CPS):
                l = n - c
                if 0 <= l < DEPTH:
                    st = _zero_state() if c == 0 else ST[(b, c - 1)]
                    in_maps.append(_layer_inputs(p, l, X[(b, c)], pos[b, c * NTOK:(c + 1) * NTOK], st))
                    active.append((b, c, l))
                else:
                    in_maps.append(_layer_inputs(p, 0, zeros_x, pos[b, c * NTOK:(c + 1) * NTOK], _zero_state()))
                    active.append(None)
        res = run_bass_kernel_spmd(nc, in_maps, core_ids=list(range(NB * CPS)))
        newST = {}
        for i, a in enumerate(active):
            if a is None:
                continue
            b, c, l = a
            r = res.results[i]
            X[(b, c)] = np.ascontiguousarray(r["x_out"])
            newST[(b, c)] = dict(s_ret=np.ascontiguousarray(r["so_ret"]), s_gla=np.ascontiguousarray(r["so_gla"]),
                                 s_ssd=np.ascontiguousarray(r["so_ssd"]), hist=np.ascontiguousarray(r["hist_out"]))
            if l == DEPTH - 1:
                out[b, c * NTOK:(c + 1) * NTOK] = r["xn_out"]
        ST = newST
    return out


def kernel(**inputs):
    p = {k: np.asarray(v) for k, v in inputs.items()}
    return run_pipeline(p, 4096, 4, 4, 2)
```
